# Optimizing a Trainium2 kernel written in Bass

```python
import math, functools
import jax, jax.numpy as jnp
from jax import lax
import numpy as np

D_MODEL = 2048
BATCH = 4
SEQ = 4096
DEPTH = 4

GRID_W = 64
CTX_LEN = 256
N_MIXERS = 4
GW = D_MODEL // N_MIXERS
CONV_K = 4

LRU_BLOCKS = 8
LRU_BLOCK = GW // LRU_BLOCKS
LRU_C = 8.0

RET_HEADS = 4
RET_HEAD_DIM = GW // RET_HEADS
RET_CHUNK = 128
ROPE_BASE = 10000.0

SSD_HEAD_DIM = 64
SSD_HEADS = GW // SSD_HEAD_DIM
SSD_GROUPS = 2
SSD_STATE = 64
SSD_CHUNK = 128

GDN_HEADS = 4
GDN_HEAD_DIM = GW // GDN_HEADS
GDN_CHUNK = 64

A_SIZES = (GW, GW)
B_SIZES = (GW, GW, GW, GW)
C_SIZES = (GW, GW + 2 * SSD_GROUPS * SSD_STATE, SSD_HEADS)
D_SIZES = (3 * GW, GW, 2 * GDN_HEADS, 2 * GDN_HEADS)
GROUP_COLS = (sum(A_SIZES), sum(B_SIZES), sum(C_SIZES), sum(D_SIZES))
IN_COLS = sum(GROUP_COLS)

N_EXPERTS = 64
TOP_K = 8
N_EXPERT_GROUPS = 8
TOPK_GROUPS = 4
D_EXPERT = 256
ROUTED_SCALE = 2.5

DEEPNORM_ALPHA = (2 * DEPTH) ** 0.25
DEEPNORM_BETA = (8 * DEPTH) ** -0.25

kernel_name = 'hymba_style_bidir_lru_ret_ssd_gdn_moe'


def _split(t, sizes):
    idx = [int(s) for s in np.cumsum(sizes)[:-1]]
    return jnp.split(t, idx, axis=-1)


def _layer_norm(t, w, b, eps=1e-5):
    tf = t.astype(jnp.float32)
    mu = jnp.mean(tf, axis=-1, keepdims=True)
    var = jnp.mean(jnp.square(tf - mu), axis=-1, keepdims=True)
    return ((tf - mu) * lax.rsqrt(var + eps) * w + b).astype(t.dtype)


def _rms_norm(t, w, eps=1e-6):
    tf = t.astype(jnp.float32)
    return (tf * lax.rsqrt(jnp.mean(tf * tf, axis=-1, keepdims=True) + eps) * w).astype(t.dtype)


def _l2norm(t, eps=1e-6):
    return t * lax.rsqrt(jnp.sum(t * t, axis=-1, keepdims=True) + eps)


def _dw_conv(t, w, b=None):
    k, ch = w.shape
    left = k // 2
    y = lax.conv_general_dilated(t, w[:, None, :].astype(t.dtype), window_strides=(1,),
                                 padding=((left, k - 1 - left),),
                                 dimension_numbers=('NWC', 'WIO', 'NWC'),
                                 feature_group_count=ch)
    return y if b is None else y + b


def _rope_1d(t, pos):
    d = t.shape[-1]
    inv = ROPE_BASE ** (-jnp.arange(0, d, 2, dtype=jnp.float32) / d)
    ang = pos[:, None] * inv[None, :]
    cos = jnp.cos(ang)[None, :, None, :]
    sin = jnp.sin(ang)[None, :, None, :]
    t1, t2 = t[..., : d // 2], t[..., d // 2:]
    return jnp.concatenate([t1 * cos - t2 * sin, t1 * sin + t2 * cos], axis=-1)


def _rope_2d(t, rows, cols):
    h = t.shape[-1] // 2
    return jnp.concatenate([_rope_1d(t[..., :h], rows), _rope_1d(t[..., h:], cols)], axis=-1)


def _chunks(t, chunk):
    b, h, T = t.shape[:3]
    return jnp.moveaxis(t.reshape(b, h, T // chunk, chunk, *t.shape[3:]), 2, 0)


def _unchunk(y):
    n, b, h, c = y.shape[:4]
    return jnp.moveaxis(y, 0, 2).reshape(b, h, n * c, *y.shape[4:])


def _decay_scan(q, k, v, log_a, s0, chunk):
    tril = jnp.tril(jnp.ones((chunk, chunk), dtype=bool))

    def step(s, inp):
        qi, ki, vi, gi = inp
        cum = jnp.cumsum(gi, axis=-1)
        decay = jnp.exp(jnp.where(tril, cum[..., :, None] - cum[..., None, :], -jnp.inf))
        scores = jnp.einsum('bhik,bhjk->bhij', qi, ki) * decay
        y = (jnp.einsum('bhij,bhjv->bhiv', scores, vi)
             + jnp.einsum('bhik,bhkv->bhiv', qi * jnp.exp(cum)[..., None], s))
        last = cum[..., -1:]
        s = (jnp.exp(last)[..., None] * s
             + jnp.einsum('bhjk,bhjv->bhkv', ki * jnp.exp(last - cum)[..., None], vi))
        return s, y

    s_fin, y = lax.scan(step, s0, tuple(_chunks(t, chunk) for t in (q, k, v, log_a)))
    return _unchunk(y), s_fin


def _gated_delta_scan(q, k, v, log_a, beta, s0, chunk):
    dv = v.shape[-1]
    tril = jnp.tril(jnp.ones((chunk, chunk), dtype=bool))
    strict = jnp.tril(jnp.ones((chunk, chunk), dtype=bool), -1)
    eye = jnp.eye(chunk, dtype=q.dtype)

    def step(s, inp):
        qi, ki, vi, gi, bi = inp
        cum = jnp.cumsum(gi, axis=-1)
        dec = jnp.exp(jnp.where(tril, cum[..., :, None] - cum[..., None, :], -jnp.inf))
        kb = ki * bi[..., None]
        m = jnp.where(strict, jnp.einsum('bhik,bhjk->bhij', kb, ki) * dec, 0.0)
        rhs = jnp.concatenate([vi * bi[..., None], kb * jnp.exp(cum)[..., None]], axis=-1)
        sol = lax.linalg.triangular_solve(eye + m, rhs, left_side=True, lower=True,
                                          unit_diagonal=True)
        u, w = sol[..., :dv], sol[..., dv:]
        v_new = u - jnp.einsum('bhik,bhkv->bhiv', w, s)
        attn = jnp.einsum('bhik,bhjk->bhij', qi, ki) * dec
        o = (jnp.einsum('bhik,bhkv->bhiv', qi * jnp.exp(cum)[..., None], s)
             + jnp.einsum('bhij,bhjv->bhiv', attn, v_new))
        last = cum[..., -1:]
        s = (jnp.exp(last)[..., None] * s
             + jnp.einsum('bhjk,bhjv->bhkv', ki * jnp.exp(last - cum)[..., None], v_new))
        return s, o

    s_fin, o = lax.scan(step, s0, tuple(_chunks(t, chunk) for t in (q, k, v, log_a, beta)))
    return _unchunk(o), s_fin


def _lin_combine(l, r):
    al, bl = l
    ar, br = r
    return al * ar, ar * bl + br


def _lru_scan(a, b, h0):
    b = b.at[:, 0].add(a[:, 0] * h0)
    _, h = lax.associative_scan(_lin_combine, (a, b), axis=1)
    return h, h[:, -1]


def _prefix_two_pass(scan_fn, ctx_in, lat_in, s0, axis, reverse):
    if reverse:
        ctx_in = tuple(jnp.flip(t, axis) for t in ctx_in)
        lat_in = tuple(jnp.flip(t, axis) for t in lat_in)
    y_ctx, s_ctx = scan_fn(*ctx_in, s0)
    y_lat, _ = scan_fn(*lat_in, s_ctx)
    if reverse:
        y_ctx, y_lat = jnp.flip(y_ctx, axis), jnp.flip(y_lat, axis)
    return y_ctx, y_lat


def _block_diag(t, w):
    bsz, T, ch = t.shape
    y = jnp.einsum('btgi,gij->btgj', t.reshape(bsz, T, LRU_BLOCKS, LRU_BLOCK), w)
    return y.reshape(bsz, T, ch)


def _lru_coeffs(xc, ga_w, ga_b, gx_w, gx_b, lam):
    r = jax.nn.sigmoid(_block_diag(xc, ga_w) + ga_b)
    i = jax.nn.sigmoid(_block_diag(xc, gx_w) + gx_b)
    log_a = -LRU_C * r * jax.nn.softplus(-lam)
    return (jnp.exp(log_a), jnp.sqrt(-jnp.expm1(2.0 * log_a)) * (i * xc))


def _rglru_mixer(p_ctx, p_lat, conv_w, conv_b, ga_w, ga_b, gx_w, gx_b, lam):
    out_dtype = p_lat.dtype

    def prep(p):
        xb, gate = jnp.split(p, 2, axis=-1)
        return _dw_conv(xb, conv_w, conv_b).astype(jnp.float32), gate

    xc_c, gate_c = prep(p_ctx)
    xc_l, gate_l = prep(p_lat)
    h0 = jnp.zeros((p_lat.shape[0], GW), jnp.float32)
    h_c = h_l = 0.0
    for d in range(2):
        y_c, y_l = _prefix_two_pass(
            _lru_scan,
            _lru_coeffs(xc_c, ga_w[d], ga_b[d], gx_w[d], gx_b[d], lam[d]),
            _lru_coeffs(xc_l, ga_w[d], ga_b[d], gx_w[d], gx_b[d], lam[d]),
            h0, axis=1, reverse=(d == 1))
        h_c = h_c + y_c
        h_l = h_l + y_l
    out_c = jax.nn.gelu(gate_c.astype(jnp.float32)) * h_c
    out_l = jax.nn.gelu(gate_l.astype(jnp.float32)) * h_l
    return out_c.astype(out_dtype), out_l.astype(out_dtype)


def _retention_mixer(p_ctx, p_lat, rows, cols, log_decay, norm_w):
    out_dtype = p_lat.dtype

    def prep(p, rotate):
        bsz, t = p.shape[:2]
        q, k, v, g = jnp.split(p.astype(jnp.float32), 4, axis=-1)
        q, k, v = (z.reshape(bsz, t, RET_HEADS, RET_HEAD_DIM) for z in (q, k, v))
        if rotate:
            q, k = _rope_2d(q, rows, cols), _rope_2d(k, rows, cols)
        q = q * RET_HEAD_DIM ** -0.5
        return tuple(jnp.swapaxes(z, 1, 2) for z in (q, k, v)), g

    in_c, g_c = prep(p_ctx, False)
    in_l, g_l = prep(p_lat, True)
    s0 = jnp.zeros((p_lat.shape[0], RET_HEADS, RET_HEAD_DIM, RET_HEAD_DIM), jnp.float32)
    scan = functools.partial(_decay_scan, chunk=RET_CHUNK)
    o_c = o_l = 0.0
    for d in range(2):
        la = -jnp.exp(log_decay[d].astype(jnp.float32))[None, :, None]
        y_c, y_l = _prefix_two_pass(
            scan, (*in_c, jnp.broadcast_to(la, in_c[0].shape[:3])),
            (*in_l, jnp.broadcast_to(la, in_l[0].shape[:3])), s0, axis=2, reverse=(d == 1))
        o_c = o_c + y_c
        o_l = o_l + y_l

    def finish(o, g):
        o = jnp.swapaxes(o, 1, 2)
        o = _rms_norm(o, norm_w.reshape(RET_HEADS, RET_HEAD_DIM))
        return (jax.nn.silu(g) * o.reshape(*o.shape[:2], GW)).astype(out_dtype)

    return finish(o_c, g_c), finish(o_l, g_l)


def _ssd_mixer(p_ctx, p_lat, conv_w, conv_b, dt_bias, a_log, d_skip, norm_w):
    out_dtype = p_lat.dtype
    rep = SSD_HEADS // SSD_GROUPS

    def prep(p):
        bsz, t = p.shape[:2]
        z, xbc, dt_raw = _split(p, C_SIZES)
        xbc = jax.nn.silu(_dw_conv(xbc, conv_w, conv_b)).astype(jnp.float32)
        xs, bm, cm = _split(xbc, (GW, SSD_GROUPS * SSD_STATE, SSD_GROUPS * SSD_STATE))
        xs = xs.reshape(bsz, t, SSD_HEADS, SSD_HEAD_DIM).swapaxes(1, 2)
        bm = jnp.repeat(bm.reshape(bsz, t, SSD_GROUPS, SSD_STATE), rep, axis=2).swapaxes(1, 2)
        cm = jnp.repeat(cm.reshape(bsz, t, SSD_GROUPS, SSD_STATE), rep, axis=2).swapaxes(1, 2)
        return z, (xs, bm, cm, dt_raw.astype(jnp.float32).swapaxes(1, 2))

    def dir_inputs(xs, bm, cm, dt_raw, d):
        dt = jax.nn.softplus(dt_raw + dt_bias[d][None, :, None])
        log_a = -jnp.exp(a_log[d])[None, :, None] * dt
        return (cm, bm, xs * dt[..., None], log_a)

    z_c, in_c = prep(p_ctx)
    z_l, in_l = prep(p_lat)
    s0 = jnp.zeros((p_lat.shape[0], SSD_HEADS, SSD_STATE, SSD_HEAD_DIM), jnp.float32)
    scan = functools.partial(_decay_scan, chunk=SSD_CHUNK)
    y_c = d_skip[None, :, None, None] * in_c[0]
    y_l = d_skip[None, :, None, None] * in_l[0]
    for d in range(2):
        a_c, a_l = _prefix_two_pass(scan, dir_inputs(*in_c, d), dir_inputs(*in_l, d), s0,
                                    axis=2, reverse=(d == 1))
        y_c = y_c + a_c
        y_l = y_l + a_l

    def finish(y, z):
        y = jnp.swapaxes(y, 1, 2).reshape(z.shape[0], z.shape[1], GW)
        return _rms_norm(y * jax.nn.silu(z.astype(jnp.float32)), norm_w).astype(out_dtype)

    return finish(y_c, z_c), finish(y_l, z_l)


def _gdn_mixer(p_ctx, p_lat, conv_w, dt_bias, a_log, norm_w):
    out_dtype = p_lat.dtype

    def prep(p):
        bsz, t = p.shape[:2]
        qkv, z, a, b = _split(p, D_SIZES)
        qkv = jax.nn.silu(_dw_conv(qkv, conv_w)).astype(jnp.float32)
        q, k, v = (u.reshape(bsz, t, GDN_HEADS, GDN_HEAD_DIM).swapaxes(1, 2)
                   for u in jnp.split(qkv, 3, axis=-1))
        q = _l2norm(q) * GDN_HEAD_DIM ** -0.5
        k = _l2norm(k)
        a = a.astype(jnp.float32).reshape(bsz, t, 2, GDN_HEADS).transpose(2, 0, 3, 1)
        b = b.astype(jnp.float32).reshape(bsz, t, 2, GDN_HEADS).transpose(2, 0, 3, 1)
        return z, (q, k, v, a, b)

    def dir_inputs(q, k, v, a, b, d):
        log_a = -jnp.exp(a_log[d])[None, :, None] * jax.nn.softplus(a[d] + dt_bias[d][None, :, None])
        return (q, k, v, log_a, jax.nn.sigmoid(b[d]))

    z_c, in_c = prep(p_ctx)
    z_l, in_l = prep(p_lat)
    s0 = jnp.zeros((p_lat.shape[0], GDN_HEADS, GDN_HEAD_DIM, GDN_HEAD_DIM), jnp.float32)
    scan = functools.partial(_gated_delta_scan, chunk=GDN_CHUNK)
    o_c = o_l = 0.0
    for d in range(2):
        y_c, y_l = _prefix_two_pass(scan, dir_inputs(*in_c, d), dir_inputs(*in_l, d), s0,
                                    axis=2, reverse=(d == 1))
        o_c = o_c + y_c
        o_l = o_l + y_l

    def finish(o, z):
        bsz, t = z.shape[:2]
        o = _rms_norm(jnp.swapaxes(o, 1, 2), norm_w)
        gz = jax.nn.silu(z.astype(jnp.float32)).reshape(bsz, t, GDN_HEADS, GDN_HEAD_DIM)
        return (o * gz).reshape(bsz, t, GW).astype(out_dtype)

    return finish(o_c, z_c), finish(o_l, z_l)


def _moe_ffn(u, router_w, router_bias, w1, w3, w2, sw1, sw3, sw2):
    shp = u.shape
    t = u.reshape(-1, D_MODEL)
    n = t.shape[0]
    scores = jax.nn.sigmoid((t @ router_w).astype(jnp.float32))
    sel = scores + router_bias.astype(jnp.float32)
    grp = sel.reshape(n, N_EXPERT_GROUPS, N_EXPERTS // N_EXPERT_GROUPS)
    grp_score = jnp.sum(lax.top_k(grp, 2)[0], axis=-1)
    _, top_groups = lax.top_k(grp_score, TOPK_GROUPS)
    gmask = jnp.sum(jax.nn.one_hot(top_groups, N_EXPERT_GROUPS, dtype=jnp.float32), axis=1)
    emask = jnp.repeat(gmask, N_EXPERTS // N_EXPERT_GROUPS, axis=1)
    _, top_idx = lax.top_k(jnp.where(emask > 0, sel, -jnp.inf), TOP_K)
    wts = jnp.take_along_axis(scores, top_idx, axis=1)
    wts = wts / jnp.sum(wts, axis=-1, keepdims=True) * ROUTED_SCALE
    gate = jnp.sum(jax.nn.one_hot(top_idx, N_EXPERTS, dtype=jnp.float32) * wts[..., None], axis=1)
    h = jax.nn.silu(jnp.einsum('nd,edf->nef', t, w1)) * jnp.einsum('nd,edf->nef', t, w3)
    routed = jnp.einsum('nef,efd->nd', h * gate[..., None].astype(h.dtype), w2)
    shared = (jax.nn.silu(t @ sw1) * (t @ sw3)) @ sw2
    return (routed + shared).reshape(shp)


def setup_inputs(seed: int = 0) -> dict:
    key = jax.random.key(seed)
    ks = iter(jax.random.split(key, 48))
    L, D = DEPTH, D_MODEL

    def nrm(shape, scale):
        return jax.random.normal(next(ks), shape, jnp.float32) * scale

    def unif(shape, lo, hi):
        return jax.random.uniform(next(ks), shape, jnp.float32, lo, hi)

    inp = {}
    inp['x'] = nrm((BATCH, SEQ, D), 1.0)
    inp['c'] = nrm((BATCH, D), 1.0)
    inp['ctx'] = nrm((BATCH, CTX_LEN, D), 1.0)
    inp['c_ctx'] = nrm((D,), 1.0)
    inp['w_ada'] = nrm((L, D, 6 * D), 0.5 * D ** -0.5)
    inp['b_ada'] = nrm((L, 6 * D), 0.01)
    inp['w_in'] = nrm((L, D, IN_COLS), D ** -0.5)
    inp['lru_conv_w'] = nrm((L, CONV_K, GW), CONV_K ** -0.5)
    inp['lru_conv_b'] = nrm((L, GW), 0.01)
    inp['lru_gate_a_w'] = nrm((L, 2, LRU_BLOCKS, LRU_BLOCK, LRU_BLOCK), LRU_BLOCK ** -0.5)
    inp['lru_gate_a_b'] = nrm((L, 2, GW), 0.01)
    inp['lru_gate_x_w'] = nrm((L, 2, LRU_BLOCKS, LRU_BLOCK, LRU_BLOCK), LRU_BLOCK ** -0.5)
    inp['lru_gate_x_b'] = nrm((L, 2, GW), 0.01)
    s = unif((L, 2, GW), 0.9, 0.999) ** (1.0 / LRU_C)
    inp['lru_lambda'] = jnp.log(s) - jnp.log1p(-s)
    base = jnp.log(-jnp.log1p(-(2.0 ** (-5.0 - jnp.arange(RET_HEADS, dtype=jnp.float32)))))
    inp['ret_log_decay'] = base + nrm((L, 2, RET_HEADS), 0.05)
    inp['ret_norm_w'] = 1.0 + nrm((L, GW), 0.05)
    ssd_xbc = GW + 2 * SSD_GROUPS * SSD_STATE
    inp['ssd_conv_w'] = nrm((L, CONV_K, ssd_xbc), CONV_K ** -0.5)
    inp['ssd_conv_b'] = nrm((L, ssd_xbc), 0.01)
    dt = jnp.exp(unif((L, 2, SSD_HEADS), math.log(1e-3), math.log(1e-1)))
    inp['ssd_dt_bias'] = dt + jnp.log(-jnp.expm1(-dt))
    inp['ssd_a_log'] = jnp.log(unif((L, 2, SSD_HEADS), 1.0, 16.0))
    inp['ssd_d'] = 1.0 + nrm((L, SSD_HEADS), 0.1)
    inp['ssd_norm_w'] = 1.0 + nrm((L, GW), 0.05)
    inp['gdn_conv_w'] = nrm((L, CONV_K, 3 * GW), CONV_K ** -0.5)
    dtg = jnp.exp(unif((L, 2, GDN_HEADS), math.log(1e-3), math.log(1e-1)))
    inp['gdn_dt_bias'] = dtg + jnp.log(-jnp.expm1(-dtg))
    inp['gdn_a_log'] = jnp.log(unif((L, 2, GDN_HEADS), 1.0, 16.0))
    inp['gdn_norm_w'] = 1.0 + nrm((L, GDN_HEAD_DIM), 0.05)
    inp['w_out'] = nrm((L, D, D), DEEPNORM_BETA * D ** -0.5)
    inp['ln1_w'] = 1.0 + nrm((L, D), 0.05)
    inp['ln1_b'] = nrm((L, D), 0.01)
    inp['router_w'] = nrm((L, D, N_EXPERTS), D ** -0.5)
    inp['router_bias'] = nrm((L, N_EXPERTS), 0.01)
    inp['exp_w1'] = nrm((L, N_EXPERTS, D, D_EXPERT), D ** -0.5)
    inp['exp_w3'] = nrm((L, N_EXPERTS, D, D_EXPERT), D ** -0.5)
    inp['exp_w2'] = nrm((L, N_EXPERTS, D_EXPERT, D), DEEPNORM_BETA * D_EXPERT ** -0.5)
    inp['sh_w1'] = nrm((L, D, D_EXPERT), D ** -0.5)
    inp['sh_w3'] = nrm((L, D, D_EXPERT), D ** -0.5)
    inp['sh_w2'] = nrm((L, D_EXPERT, D), DEEPNORM_BETA * D_EXPERT ** -0.5)
    inp['ln2_w'] = 1.0 + nrm((L, D), 0.05)
    inp['ln2_b'] = nrm((L, D), 0.01)
    return inp


def reference(x, c, ctx, c_ctx, w_ada, b_ada, w_in, lru_conv_w, lru_conv_b, lru_gate_a_w,
              lru_gate_a_b, lru_gate_x_w, lru_gate_x_b, lru_lambda, ret_log_decay, ret_norm_w,
              ssd_conv_w, ssd_conv_b, ssd_dt_bias, ssd_a_log, ssd_d, ssd_norm_w, gdn_conv_w,
              gdn_dt_bias, gdn_a_log, gdn_norm_w, w_out, ln1_w, ln1_b, router_w, router_bias,
              exp_w1, exp_w3, exp_w2, sh_w1, sh_w3, sh_w2, ln2_w, ln2_b):
    T = x.shape[1]
    ROWS = T // GRID_W
    rows = jnp.repeat(jnp.arange(ROWS, dtype=jnp.float32), GRID_W)
    cols = jnp.tile(jnp.arange(GRID_W, dtype=jnp.float32), ROWS)
    silu_c = jax.nn.silu(c)
    silu_cc = jax.nn.silu(c_ctx)
    x_lat, x_ctx = x, ctx
    for l in range(DEPTH):
        mod_lat = (silu_c @ w_ada[l] + b_ada[l])[:, None, :]
        mod_ctx = (silu_cc @ w_ada[l] + b_ada[l])[None, None, :]
        sh1, sc1, g1, sh2, sc2, g2 = jnp.split(mod_lat, 6, axis=-1)
        csh1, csc1, cg1, csh2, csc2, cg2 = jnp.split(mod_ctx, 6, axis=-1)

        p_lat = (x_lat * (1.0 + sc1) + sh1) @ w_in[l]
        p_ctx = (x_ctx * (1.0 + csc1) + csh1) @ w_in[l]
        pa_l, pb_l, pc_l, pd_l = _split(p_lat, GROUP_COLS)
        pa_c, pb_c, pc_c, pd_c = _split(p_ctx, GROUP_COLS)

        ya_c, ya_l = _rglru_mixer(pa_c, pa_l, lru_conv_w[l], lru_conv_b[l], lru_gate_a_w[l],
                                  lru_gate_a_b[l], lru_gate_x_w[l], lru_gate_x_b[l], lru_lambda[l])
        yb_c, yb_l = _retention_mixer(pb_c, pb_l, rows, cols, ret_log_decay[l], ret_norm_w[l])
        yc_c, yc_l = _ssd_mixer(pc_c, pc_l, ssd_conv_w[l], ssd_conv_b[l], ssd_dt_bias[l],
                                ssd_a_log[l], ssd_d[l], ssd_norm_w[l])
        yd_c, yd_l = _gdn_mixer(pd_c, pd_l, gdn_conv_w[l], gdn_dt_bias[l], gdn_a_log[l], gdn_norm_w[l])

        y_lat = jnp.concatenate([ya_l, yb_l, yc_l, yd_l], axis=-1) @ w_out[l]
        x_lat = _layer_norm(DEEPNORM_ALPHA * x_lat + g1 * y_lat, ln1_w[l], ln1_b[l])
        f_lat = _moe_ffn(x_lat * (1.0 + sc2) + sh2, router_w[l], router_bias[l], exp_w1[l],
                         exp_w3[l], exp_w2[l], sh_w1[l], sh_w3[l], sh_w2[l])
        x_lat = _layer_norm(DEEPNORM_ALPHA * x_lat + g2 * f_lat, ln2_w[l], ln2_b[l])

        if l < DEPTH - 1:
            y_ctx = jnp.concatenate([ya_c, yb_c, yc_c, yd_c], axis=-1) @ w_out[l]
            x_ctx = _layer_norm(DEEPNORM_ALPHA * x_ctx + cg1 * y_ctx, ln1_w[l], ln1_b[l])
            f_ctx = _moe_ffn(x_ctx * (1.0 + csc2) + csh2, router_w[l], router_bias[l], exp_w1[l],
                             exp_w3[l], exp_w2[l], sh_w1[l], sh_w3[l], sh_w2[l])
            x_ctx = _layer_norm(DEEPNORM_ALPHA * x_ctx + cg2 * f_ctx, ln2_w[l], ln2_b[l])
    return x_lat
```

```python
import os
import numpy as np
from contextlib import ExitStack
import concourse.bass as bass
import concourse.mybir as mybir
from concourse.bass_utils import run_bass_kernel_spmd

F32 = mybir.dt.float32
BF16 = mybir.dt.bfloat16
AF = mybir.ActivationFunctionType
ALU = mybir.AluOpType
AX = mybir.AxisListType

D = 2048
DEPTH = 4
NB = 4
SEQ = 4096
CTX = 256
GW = 512
NE = 64
DE = 256
ALPHA = (2 * DEPTH) ** 0.25
NCORES = 8


class KB:
    EPOCH = 50000
    NDMA = 24

    def __init__(self, nc):
        self.nc = nc
        self.es = ExitStack()
        self.eng = {'pe': nc.tensor, 'dve': nc.vector, 'act': nc.scalar,
                    'pool': nc.gpsimd, 'sp': nc.sync}
        self.cur = {}
        self.waited = {}
        self.lastw = {}
        self.readers = {}
        self.dsem = [self.es.enter_context(nc.semaphore("dma%d" % i)) for i in range(self.NDMA)]
        self.dcnt = [0] * self.NDMA
        self.drr = 0
        self.nsem = 0
        self.out_toks = []
        self.nops = 0

    def sb(self, name, shape, dt=F32):
        return self.es.enter_context(self.nc.sbuf_tensor(name, shape, dt))

    def ps(self, name, shape, dt=F32):
        return self.es.enter_context(self.nc.psum_tensor(name, shape, dt))

    def _tok(self, eng):
        c = self.cur.get(eng)
        if c is None or c[1] >= self.EPOCH:
            sem = self.es.enter_context(self.nc.semaphore("s_%s_%d" % (eng, self.nsem)))
            self.nsem += 1
            c = [sem, 0]
            self.cur[eng] = c
        c[1] += 1
        return (c[0], c[1], eng)

    def _wait(self, eng, tok):
        sem, val, src = tok
        if src == eng and (eng == 'pe' or os.environ.get('KB_SELF', '1') == '0'):
            return
        k = (eng, id(sem))
        if self.waited.get(k, 0) >= val:
            return
        self.eng[eng].wait_ge(sem, val)
        self.waited[k] = val

    def _deps(self, eng, reads, writes):
        for k in reads:
            t = self.lastw.get(k)
            if t is not None:
                self._wait(eng, t)
            if k.startswith('ps'):
                for t in self.readers.get(k, {}).values():
                    if t[2] != eng:
                        self._wait(eng, t)
        for k in writes:
            t = self.lastw.get(k)
            if t is not None:
                self._wait(eng, t)
            for t in self.readers.get(k, {}).values():
                self._wait(eng, t)

    def _record(self, tok, reads, writes):
        for k in reads:
            d = self.readers.setdefault(k, {})
            kk = id(tok[0])
            if kk not in d or d[kk][1] < tok[1]:
                d[kk] = tok
        for k in writes:
            self.lastw[k] = tok
            self.readers[k] = {}

    def op(self, eng, fn, reads=(), writes=()):
        self._deps(eng, reads, writes)
        ins = fn(self.eng[eng])
        tok = self._tok(eng)
        ins.then_inc(tok[0], 1)
        self._record(tok, reads, writes)
        self.nops += 1

    def dma(self, q, out, in_, reads=(), writes=(), is_out=False):
        self._deps(q, reads, writes)
        i = self.drr
        self.drr = (self.drr + 1) % self.NDMA
        sem = self.dsem[i]
        if self.dcnt[i] > 0:
            self._wait(q, (sem, self.dcnt[i], 'dma'))
        ins = self.eng[q].dma_start(out=out, in_=in_)
        self.dcnt[i] += 16
        ins.then_inc(sem, 16)
        tok = (sem, self.dcnt[i], 'dma')
        self._record(tok, reads, writes)
        if is_out:
            self.out_toks.append(tok)
        self.nops += 1
        return tok

    def barrier(self):
        engs = ['pe', 'dve', 'act', 'pool', 'sp']
        toks = []
        for e in engs:
            c = self.cur.get(e)
            if c is not None and c[1] > 0:
                toks.append((c[0], c[1], e))
        for i in range(self.NDMA):
            if self.dcnt[i] > 0:
                toks.append((self.dsem[i], self.dcnt[i], 'dma'))
        for e in engs:
            for t in toks:
                if t[2] == e:
                    continue
                self._wait(e, t)
        self.lastw = {}
        self.readers = {}

    def finish(self):
        for t in self.out_toks:
            self._wait('sp', t)
        self.barrier()
        self.es.close()


def _consts_np():
    ident = np.eye(128, dtype=np.float32)
    ones = np.ones((128, 128), dtype=np.float32)
    return ident, ones


def build_A():
    nc = bass.Bass("TRN2", target_bir_lowering=False)
    w = nc.dram_tensor("w", [D, 6144], F32, kind="ExternalInput").ap()
    bias = nc.dram_tensor("bias", [128, 48], F32, kind="ExternalInput").ap()
    cvT = nc.dram_tensor("cvT", [128, 16, 5], F32, kind="ExternalInput").ap()
    out = nc.dram_tensor("out", [128, 48, 5], F32, kind="ExternalOutput").ap()
    kb = KB(nc)
    cv = kb.sb("cv", [128, 16, 5])
    bt = kb.sb("bt", [128, 48])
    ot = kb.sb("ot", [128, 48, 5])
    wb = [kb.sb("wb%d" % i, [128, 16, 128]) for i in range(3)]
    ps = [kb.ps("ps%d" % i, [128, 512]) for i in range(2)]
    kb.dma('sp', cv[:], cvT, writes=['cv'])
    kb.dma('sp', bt[:], bias, writes=['bt'])
    kb.op('act', lambda e: e.activation(out=cv[:], in_=cv[:], func=AF.Silu), reads=['cv'], writes=['cv'])
    wv = w.rearrange("(k p) c -> p k c", p=128)
    for cc in range(48):
        b = cc % 3
        kb.dma('sp' if cc % 2 == 0 else 'act', wb[b][:], wv[:, :, cc * 128:(cc + 1) * 128], writes=['wb%d' % b])
        pb = cc % 2
        for k in range(16):
            kb.op('pe', lambda e: e.matmul(ps[pb][:, 0:5], lhsT=wb[b][:, k, :], rhs=cv[:, k, :],
                                           start=(k == 0), stop=(k == 15)),
                  reads=['wb%d' % b, 'cv'], writes=['ps%d' % pb])
        kb.op('dve', lambda e: e.tensor_scalar(out=ot[:, cc, :], in0=ps[pb][:, 0:5], scalar1=bt[:, cc:cc + 1],
                                               scalar2=None, op0=ALU.add),
              reads=['ps%d' % pb, 'bt'], writes=['ot'])
    kb.dma('sp', out, ot[:], reads=['ot'], is_out=True)
    kb.finish()
    return nc


TT = 2176
T_GROUPS = [(0, 128, 0), (128, 512, 1), (640, 512, 1), (1152, 512, 1), (1664, 512, 1)]
T_PASSES = [[0, 1, 2], [3, 4]]
PT = 1152


def build_T():
    nc = bass.Bass("TRN2", target_bir_lowering=False)
    dr = lambda n, s, k="ExternalInput": nc.dram_tensor(n, s, F32, kind=k).ap()
    ycT = dr("ycT", [D, TT])
    xT = dr("xT", [D, TT])
    modp_d = dr("modp", [128, 2 * 4 * 16])
    lnp_d = dr("lnp", [128, 68])
    w_out = dr("w_out", [D, D])
    rw_d = dr("rw", [D, NE])
    rb_d = dr("rb", [128, NE])
    ew1 = dr("ew1", [NE + 1, D, DE])
    ew3 = dr("ew3", [NE + 1, D, DE])
    ew2 = dr("ew2", [NE + 1, DE, D])
    ident_d = dr("ident", [128, 128])
    x1s = dr("x1s", [D, TT], "Internal")
    x2T = dr("x2T", [D, TT], "ExternalOutput")

    kb = KB(nc)
    Wr = kb.sb("Wr", [128, 24576], BF16)
    Fr = kb.sb("Fr", [128, 18432], F32)
    Ur = kb.sb("Ur", [128, 18432], BF16)
    modp = kb.sb("modp_t", [128, 2, 4, 16])
    lnp = kb.sb("lnp_t", [128, 68])
    rw = kb.sb("rw_t", [128, 16, NE])
    rb = kb.sb("rb_t", [128, NE])
    ident = kb.sb("ident_t", [128, 128])
    ones = kb.sb("ones_t", [128, 128])
    sq = [kb.sb("sq%d" % i, [128, 512]) for i in range(2)]
    mn = kb.sb("mn", [128, 512])
    rs = kb.sb("rs", [128, 512])
    t1 = kb.sb("t1", [128, 512])
    gateT = kb.sb("gateT", [128, PT])
    hT = kb.sb("hT", [128, 2, 512], BF16)
    sbuf_s = [kb.sb("s_s%d" % i, [128, 512]) for i in range(2)]
    sbuf_t = [kb.sb("s_t%d" % i, [128, 512]) for i in range(2)]
    r_sc = kb.sb("r_sc", [128, 64]); r_sel = kb.sb("r_sel", [128, 64]); r_m8 = kb.sb("r_m8", [128, 64])
    r_gs = kb.sb("r_gs", [128, 8]); r_g8 = kb.sb("r_g8", [128, 8]); r_gm = kb.sb("r_gm", [128, 8])
    r_em = kb.sb("r_em", [128, 64]); r_tmp = kb.sb("r_tmp", [128, 64]); r_t8 = kb.sb("r_t8", [128, 8])
    r_sum = kb.sb("r_sum", [128, 1]); r_gate = kb.sb("r_gate", [128, 64])
    PS = [kb.ps("ps%d" % i, [128, 512]) for i in range(8)]

    ycb = Wr[:, 0:8192].rearrange("p (c t) -> p c t", c=16)
    ys = Wr[:, 8192:12288].bitcast(F32).rearrange("p (c t) -> p c t", c=4)
    wob = [Wr[:, 12288 + i * 2048:12288 + (i + 1) * 2048].rearrange("p (k c) -> p k c", k=16) for i in range(2)]
    xg = Fr[:, 0:8192].rearrange("p (c t) -> p c t", c=16)
    u32 = Fr[:, 8192:16384].rearrange("p (c t) -> p c t", c=16)
    facc = Fr[:, 0:16 * PT].rearrange("p (c t) -> p c t", c=16)
    uT = Ur[:, 0:16 * PT].rearrange("p (c t) -> p c t", c=16)
    x1g = Ur[:, 0:16384].bitcast(F32).rearrange("p (c t) -> p c t", c=16)
    wE = []
    for b in range(2):
        o = b * 12288
        wE.append((Wr[:, o:o + 4096].rearrange("p (k f) -> p k f", k=16),
                   Wr[:, o + 4096:o + 8192].rearrange("p (k f) -> p k f", k=16),
                   Wr[:, o + 8192:o + 12288].rearrange("p (c d) -> p c d", c=2)))

    ycv = ycT.rearrange("(c p) t -> p c t", p=128)
    xv = xT.rearrange("(c p) t -> p c t", p=128)
    x1v = x1s.rearrange("(c p) t -> p c t", p=128)
    x2v = x2T.rearrange("(c p) t -> p c t", p=128)
    wov = w_out.rearrange("(k p) d -> p k d", p=128)

    kb.dma('sp', modp[:].rearrange("p a b c -> p (a b c)"), modp_d, writes=['modp'])
    kb.dma('sp', lnp[:], lnp_d, writes=['lnp'])
    kb.dma('sp', rw[:], rw_d.rearrange("(k p) e -> p k e", p=128), writes=['rw'])
    kb.dma('sp', rb[:], rb_d, writes=['rb'])
    kb.dma('sp', ident[:], ident_d, writes=['ident'])
    kb.op('dve', lambda e: e.memset(ones[:], 1.0), writes=['ones'])
    kb.op('dve', lambda e: e.tensor_scalar_add(out=modp[:, :, 1, :], in0=modp[:, :, 1, :], scalar1=1.0),
          reads=['modp'], writes=['modp'])

    def ln_stats(src, n, keyf):
        for dc in range(16):
            kb.op('pe', lambda e: e.matmul(PS[2][:, :n], lhsT=ones[:], rhs=src[:, dc, :n], start=(dc == 0), stop=(dc == 15)),
                  reads=['ones', keyf(dc)], writes=['ps2'])
            s = sq[dc % 2]
            kb.op('act', lambda e: e.activation(out=s[:, :n], in_=src[:, dc, :n], func=AF.Square),
                  reads=[keyf(dc)], writes=['sq%d' % (dc % 2)])
            kb.op('pe', lambda e: e.matmul(PS[3][:, :n], lhsT=ones[:], rhs=s[:, :n], start=(dc == 0), stop=(dc == 15)),
                  reads=['ones', 'sq%d' % (dc % 2)], writes=['ps3'])
        kb.op('dve', lambda e: e.tensor_scalar(out=mn[:, :n], in0=PS[2][:, :n], scalar1=1.0 / D, scalar2=None, op0=ALU.mult),
              reads=['ps2'], writes=['mn'])
        kb.op('dve', lambda e: e.tensor_tensor(out=t1[:, :n], in0=mn[:, :n], in1=mn[:, :n], op=ALU.mult),
              reads=['mn'], writes=['t1'])
        kb.op('dve', lambda e: e.scalar_tensor_tensor(out=rs[:, :n], in0=PS[3][:, :n], scalar=1.0 / D, in1=t1[:, :n],
                                                      op0=ALU.mult, op1=ALU.subtract),
              reads=['ps3', 't1'], writes=['rs'])
        kb.op('dve', lambda e: e.tensor_scalar(out=rs[:, :n], in0=rs[:, :n], scalar1=0.0, scalar2=1e-5, op0=ALU.max, op1=ALU.add),
              reads=['rs'], writes=['rs'])
        kb.op('act', lambda e: e.activation(out=rs[:, :n], in_=rs[:, :n], func=AF.Sqrt), reads=['rs'], writes=['rs'])
        kb.op('dve', lambda e: e.reciprocal(out=rs[:, :n], in_=rs[:, :n]), reads=['rs'], writes=['rs'])

    def ln_apply(src, n, keyf, wcol, bcol):
        for dc in range(16):
            kb.op('dve', lambda e: e.tensor_tensor(out=src[:, dc, :n], in0=src[:, dc, :n], in1=mn[:, :n], op=ALU.subtract),
                  reads=[keyf(dc), 'mn'], writes=[keyf(dc)])
            kb.op('pool', lambda e: e.tensor_tensor(out=src[:, dc, :n], in0=src[:, dc, :n], in1=rs[:, :n], op=ALU.mult),
                  reads=[keyf(dc), 'rs'], writes=[keyf(dc)])
            kb.op('act', lambda e: e.activation(out=src[:, dc, :n], in_=src[:, dc, :n], func=AF.Identity,
                                                scale=lnp[:, wcol + dc:wcol + dc + 1], bias=lnp[:, bcol + dc:bcol + dc + 1]),
                  reads=[keyf(dc), 'lnp'], writes=[keyf(dc)])

    def phase_T1(gi, pt0):
        t0, n, s = T_GROUPS[gi]
        po = t0 - pt0
        xk = lambda dc: 'xg%d' % dc
        kb.dma('sp', xg[:, :, :n], xv[:, :, t0:t0 + n], writes=[xk(dc) for dc in range(16)])
        kb.dma('pool', ycb[:, 0:8, :n], ycv[:, 0:8, t0:t0 + n], writes=['ycbA'])
        kb.dma('pool', ycb[:, 12:16, :n], ycv[:, 12:16, t0:t0 + n], writes=['ycbB'])
        kb.dma('sp', ys[:, :, :n], ycv[:, 8:12, t0:t0 + n], writes=['ys'])
        for c in range(4):
            s_ = sq[c % 2]
            kb.op('act', lambda e: e.activation(out=s_[:, :n], in_=ys[:, c, :n], func=AF.Square),
                  reads=['ys'], writes=['sq%d' % (c % 2)])
            kb.op('pe', lambda e: e.matmul(PS[4][:, :n], lhsT=ones[:], rhs=s_[:, :n], start=(c == 0), stop=(c == 3)),
                  reads=['ones', 'sq%d' % (c % 2)], writes=['ps4'])
        kb.op('dve', lambda e: e.tensor_scalar(out=rs[:, :n], in0=PS[4][:, :n], scalar1=1.0 / GW, scalar2=1e-6,
                                               op0=ALU.mult, op1=ALU.add), reads=['ps4'], writes=['rs'])
        kb.op('act', lambda e: e.activation(out=rs[:, :n], in_=rs[:, :n], func=AF.Sqrt), reads=['rs'], writes=['rs'])
        kb.op('dve', lambda e: e.reciprocal(out=rs[:, :n], in_=rs[:, :n]), reads=['rs'], writes=['rs'])
        for c in range(4):
            kb.op('dve', lambda e: e.scalar_tensor_tensor(out=ycb[:, 8 + c, :n], in0=ys[:, c, :n], scalar=lnp[:, 64 + c:65 + c],
                                                          in1=rs[:, :n], op0=ALU.mult, op1=ALU.mult),
                  reads=['ys', 'rs', 'lnp'], writes=['ycbS'])
        for dc in range(16):
            b = dc % 2
            kb.dma('pool', wob[b], wov[:, :, dc * 128:(dc + 1) * 128], writes=['wob%d' % b])
            for k in range(16):
                kb.op('pe', lambda e: e.matmul(PS[b][:, :n], lhsT=wob[b][:, k, :], rhs=ycb[:, k, :n],
                                               start=(k == 0), stop=(k == 15)),
                      reads=['wob%d' % b, 'ycbA', 'ycbB', 'ycbS'], writes=['ps%d' % b])
            kb.op('act', lambda e: e.mul(out=xg[:, dc, :n], in_=xg[:, dc, :n], mul=ALPHA), reads=[xk(dc)], writes=[xk(dc)])
            kb.op('dve', lambda e: e.scalar_tensor_tensor(out=xg[:, dc, :n], in0=PS[b][:, :n], scalar=modp[:, s, 0, dc:dc + 1],
                                                          in1=xg[:, dc, :n], op0=ALU.mult, op1=ALU.add),
                  reads=['ps%d' % b, 'modp', xk(dc)], writes=[xk(dc)])
        ln_stats(xg, n, xk)
        ln_apply(xg, n, xk, 0, 16)
        kb.dma('sp', x1v[:, :, t0:t0 + n], xg[:, :, :n], reads=[xk(dc) for dc in range(16)], writes=['x1s%d' % gi])
        for dc in range(16):
            kb.op('act', lambda e: e.activation(out=u32[:, dc, :n], in_=xg[:, dc, :n], func=AF.Identity,
                                                scale=modp[:, s, 1, dc:dc + 1], bias=modp[:, s, 2, dc:dc + 1]),
                  reads=[xk(dc), 'modp'], writes=['u32_%d' % dc])
            kb.op('pool', lambda e: e.tensor_copy(out=uT[:, dc, po:po + n], in_=u32[:, dc, :n]),
                  reads=['u32_%d' % dc], writes=['uT'])
        for tt in range(n // 128):
            a = tt * 128
            for k in range(16):
                kb.op('pe', lambda e: e.matmul(PS[5][:, 0:NE], lhsT=u32[:, k, a:a + 128], rhs=rw[:, k, :],
                                               start=(k == 0), stop=(k == 15)),
                      reads=['u32_%d' % k, 'rw'], writes=['ps5'])
            kb.op('act', lambda e: e.activation(out=r_sc[:], in_=PS[5][:, 0:NE], func=AF.Sigmoid), reads=['ps5'], writes=['r_sc'])
            kb.op('dve', lambda e: e.tensor_tensor(out=r_sel[:], in0=r_sc[:], in1=rb[:], op=ALU.add),
                  reads=['r_sc', 'rb'], writes=['r_sel'])
            for g in range(8):
                kb.op('dve', lambda e: e.max(out=r_m8[:, g * 8:(g + 1) * 8], in_=r_sel[:, g * 8:(g + 1) * 8]),
                      reads=['r_sel'], writes=['r_m8'])
            m8v = r_m8[:].rearrange("p (g k) -> p g k", k=8)
            kb.op('dve', lambda e: e.tensor_tensor(out=r_gs[:], in0=m8v[:, :, 0], in1=m8v[:, :, 1], op=ALU.add),
                  reads=['r_m8'], writes=['r_gs'])
            kb.op('dve', lambda e: e.max(out=r_g8[:], in_=r_gs[:]), reads=['r_gs'], writes=['r_g8'])
            kb.op('dve', lambda e: e.tensor_scalar(out=r_gm[:], in0=r_gs[:], scalar1=r_g8[:, 3:4], scalar2=None, op0=ALU.is_ge),
                  reads=['r_gs', 'r_g8'], writes=['r_gm'])
            kb.op('dve', lambda e: e.tensor_copy(out=r_em[:].rearrange("p (g k) -> p g k", k=8),
                                                 in_=r_gm[:].unsqueeze(2).to_broadcast([128, 8, 8])),
                  reads=['r_gm'], writes=['r_em'])
            kb.op('dve', lambda e: e.tensor_scalar(out=r_tmp[:], in0=r_em[:], scalar1=-1.0, scalar2=1e30, op0=ALU.add, op1=ALU.mult),
                  reads=['r_em'], writes=['r_tmp'])
            kb.op('dve', lambda e: e.tensor_tensor(out=r_sel[:], in0=r_sel[:], in1=r_em[:], op=ALU.mult),
                  reads=['r_sel', 'r_em'], writes=['r_sel'])
            kb.op('dve', lambda e: e.tensor_tensor(out=r_sel[:], in0=r_sel[:], in1=r_tmp[:], op=ALU.add),
                  reads=['r_sel', 'r_tmp'], writes=['r_sel'])
            kb.op('dve', lambda e: e.max(out=r_t8[:], in_=r_sel[:]), reads=['r_sel'], writes=['r_t8'])
            kb.op('dve', lambda e: e.tensor_scalar(out=r_tmp[:], in0=r_sel[:], scalar1=r_t8[:, 7:8], scalar2=None, op0=ALU.is_ge),
                  reads=['r_sel', 'r_t8'], writes=['r_tmp'])
            kb.op('dve', lambda e: e.tensor_tensor(out=r_gate[:], in0=r_sc[:], in1=r_tmp[:], op=ALU.mult),
                  reads=['r_sc', 'r_tmp'], writes=['r_gate'])
            kb.op('dve', lambda e: e.reduce_sum(out=r_sum[:], in_=r_gate[:], axis=AX.X), reads=['r_gate'], writes=['r_sum'])
            kb.op('dve', lambda e: e.reciprocal(out=r_sum[:], in_=r_sum[:]), reads=['r_sum'], writes=['r_sum'])
            kb.op('dve', lambda e: e.tensor_scalar(out=r_gate[:], in0=r_gate[:], scalar1=r_sum[:, 0:1], scalar2=2.5,
                                                   op0=ALU.mult, op1=ALU.mult), reads=['r_gate', 'r_sum'], writes=['r_gate'])
            kb.op('pe', lambda e: e.transpose(out=PS[6][0:NE, 0:128], in_=r_gate[:], identity=ident[:]),
                  reads=['r_gate', 'ident'], writes=['ps6'])
            kb.op('act', lambda e: e.copy(out=gateT[0:NE, po + a:po + a + 128], in_=PS[6][0:NE, 0:128]),
                  reads=['ps6'], writes=['gateT'])

    def phase_T2(groups, pt0):
        kb.op('dve', lambda e: e.memset(gateT[64:65, :], 1.0), writes=['gateT'])
        segs = []
        for gi in groups:
            t0, n, s = T_GROUPS[gi]
            segs.append((t0 - pt0, n))

        def load(e):
            b = e % 2
            w1, w3, w2 = wE[b]
            kb.dma('pool', w1, ew1[e].rearrange("(k p) f -> p k f", p=128), writes=['w1_%d' % b])
            kb.dma('pool', w3, ew3[e].rearrange("(k p) f -> p k f", p=128), writes=['w3_%d' % b])
            kb.dma('pool', w2, ew2[e].rearrange("(c p) d -> p c d", p=128), writes=['w2_%d' % b])

        load(0)
        fi = 0
        for e in range(NE + 1):
            if e + 1 <= NE:
                load(e + 1)
            b = e % 2
            w1, w3, w2 = wE[b]
            for (po, n) in segs:
                kb.op('pe', lambda en: en.matmul(PS[4][:, :n], lhsT=ident[0:NE + 1, e:e + 1].to_broadcast([NE + 1, 128]),
                                                 rhs=gateT[0:NE + 1, po:po + n], start=True, stop=True),
                      reads=['ident', 'gateT'], writes=['ps4'])
                for fc in range(2):
                    for k in range(16):
                        kb.op('pe', lambda en: en.matmul(PS[fc][:, :n], lhsT=w1[:, k, fc * 128:(fc + 1) * 128], rhs=uT[:, k, po:po + n],
                                                         start=(k == 0), stop=(k == 15)),
                              reads=['w1_%d' % b, 'uT'], writes=['ps%d' % fc])
                    for k in range(16):
                        kb.op('pe', lambda en: en.matmul(PS[2 + fc][:, :n], lhsT=w3[:, k, fc * 128:(fc + 1) * 128], rhs=uT[:, k, po:po + n],
                                                         start=(k == 0), stop=(k == 15)),
                              reads=['w3_%d' % b, 'uT'], writes=['ps%d' % (2 + fc)])
                    kb.op('act', lambda en: en.activation(out=sbuf_s[fc][:, :n], in_=PS[fc][:, :n], func=AF.Silu),
                          reads=['ps%d' % fc], writes=['s_s%d' % fc])
                    kb.op('dve', lambda en: en.tensor_tensor(out=sbuf_t[fc][:, :n], in0=sbuf_s[fc][:, :n], in1=PS[2 + fc][:, :n], op=ALU.mult),
                          reads=['s_s%d' % fc, 'ps%d' % (2 + fc)], writes=['s_t%d' % fc])
                    kb.op('dve', lambda en: en.tensor_tensor(out=hT[:, fc, :n], in0=sbuf_t[fc][:, :n], in1=PS[4][:, :n], op=ALU.mult),
                          reads=['s_t%d' % fc, 'ps4'], writes=['hT%d' % fc])
                for dc in range(16):
                    pb = 5 + (fi % 3)
                    fi += 1
                    for fc in range(2):
                        kb.op('pe', lambda en: en.matmul(PS[pb][:, :n], lhsT=w2[:, fc, dc * 128:(dc + 1) * 128], rhs=hT[:, fc, :n],
                                                         start=(fc == 0), stop=(fc == 1)),
                              reads=['w2_%d' % b, 'hT%d' % fc], writes=['ps%d' % pb])
                    fk = 'f%d_%d' % (dc, po)
                    if e == 0:
                        kb.op('dve', lambda en: en.tensor_copy(out=facc[:, dc, po:po + n], in_=PS[pb][:, :n]),
                              reads=['ps%d' % pb], writes=[fk])
                    else:
                        kb.op('dve', lambda en: en.tensor_tensor(out=facc[:, dc, po:po + n], in0=PS[pb][:, :n], in1=facc[:, dc, po:po + n], op=ALU.add),
                              reads=['ps%d' % pb, fk], writes=[fk])

    def phase_T3(gi, pt0):
        t0, n, s = T_GROUPS[gi]
        po = t0 - pt0
        xk = lambda dc: 'x1g%d' % dc
        kb.dma('sp', x1g[:, :, :n], x1v[:, :, t0:t0 + n], writes=[xk(dc) for dc in range(16)])
        for dc in range(16):
            kb.op('act', lambda e: e.mul(out=x1g[:, dc, :n], in_=x1g[:, dc, :n], mul=ALPHA), reads=[xk(dc)], writes=[xk(dc)])
            kb.op('dve', lambda e: e.scalar_tensor_tensor(out=x1g[:, dc, :n], in0=facc[:, dc, po:po + n], scalar=modp[:, s, 3, dc:dc + 1],
                                                          in1=x1g[:, dc, :n], op0=ALU.mult, op1=ALU.add),
                  reads=['facc', 'modp', xk(dc)], writes=[xk(dc)])
        ln_stats(x1g, n, xk)
        ln_apply(x1g, n, xk, 32, 48)
        kb.dma('sp', x2v[:, :, t0:t0 + n], x1g[:, :, :n], reads=[xk(dc) for dc in range(16)], is_out=True)

    for groups in T_PASSES:
        pt0 = T_GROUPS[groups[0]][0]
        for gi in groups:
            phase_T1(gi, pt0)
        kb.barrier()
        phase_T2(groups, pt0)
        kb.barrier()
        for gi in groups:
            phase_T3(gi, pt0)
        kb.barrier()
    kb.finish()
    return nc


NT = CTX + SEQ
NCH = NT // 128
NFM = 28
NTM = 268
NCOLS = NFM * 128 + NTM
M_BLOCKS = [(0, 256, 0)] + [(256 + i * 512, 512, 1) for i in range(8)]
SEGS = [(0, CTX), (CTX, NT)]
PP_LRU = 0
PP_RETN = 22
PP_SSDCW = 24
PP_SSDCB = 40
PP_SSDD = 44
PP_GDNCW = 46
PP_GDNN = 70
PP_RETLD = 71
PP_SSDDTB = 75
PP_SSDAL = 83
PP_GDNDTB = 91
PP_GDNAL = 95
NPP = 99
C_IDENT, C_ONES, C_TRIF, C_TRIR, C_NMF, C_NMR, C_NSF, C_NSR = range(8)


def build_M(stages=('in', 'lru', 'ret', 'ssd', 'gdn')):
    nc = bass.Bass("TRN2", target_bir_lowering=False)
    dr = lambda n, s, k="ExternalInput": nc.dram_tensor(n, s, F32, kind=k).ap()
    xT = dr("xT", [D, NT])
    modm_d = dr("modm", [128, 2 * 2 * 16])
    w_in = dr("w_in", [D, NCOLS])
    pp_d = dr("pp", [128, NPP])
    lruw_d = dr("lruw", [128, 8, 128])
    cm_d = dr("cm", [128, 8, 128])
    ropec_d = dr("ropec", [128, NT])
    ropes_d = dr("ropes", [128, NT])
    Pfm = dr("Pfm", [NFM * 128, NT], "Internal")
    Ptm = dr("Ptm", [NT, NTM], "Internal")
    yo = dr("yo", [1024, NT], "ExternalOutput")

    kb = KB(nc)
    L = NT
    RS = 39936
    R = kb.sb("R", [128, RS], F32)
    pp = kb.sb("pp_t", [128, NPP])
    modm = kb.sb("modm_t", [128, 2, 2, 16])
    lruw = kb.sb("lruw_t", [128, 8, 128])
    cm = kb.sb("cm_t", [128, 8, 128])
    cst = kb.sb("cst", [128, 4])
    ev = [kb.sb("ev%d" % i, [128, 512]) for i in range(3)]
    tmpc = kb.sb("tmpc", [128, 16])
    PS = [kb.ps("ps%d" % i, [128, 512]) for i in range(8)]

    def tt(eng, out, in0, in1, op, r, w):
        kb.op(eng, lambda e: e.tensor_tensor(out=out, in0=in0, in1=in1, op=op), reads=r, writes=w)

    def ts(eng, out, in0, s1, s2, op0, op1, r, w):
        if s2 is None:
            kb.op(eng, lambda e: e.tensor_scalar(out=out, in0=in0, scalar1=s1, scalar2=None, op0=op0), reads=r, writes=w)
        else:
            kb.op(eng, lambda e: e.tensor_scalar(out=out, in0=in0, scalar1=s1, scalar2=s2, op0=op0, op1=op1), reads=r, writes=w)

    def stt(eng, out, in0, sc, in1, op0, op1, r, w):
        kb.op(eng, lambda e: e.scalar_tensor_tensor(out=out, in0=in0, scalar=sc, in1=in1, op0=op0, op1=op1), reads=r, writes=w)

    def act(out, in_, func, r, w, scale=1.0, bias=None):
        if bias is None:
            kb.op('act', lambda e: e.activation(out=out, in_=in_, func=func, scale=scale), reads=r, writes=w)
        else:
            kb.op('act', lambda e: e.activation(out=out, in_=in_, func=func, scale=scale, bias=bias), reads=r, writes=w)

    def mm(out, lhsT, rhs, r, w, start=True, stop=True):
        kb.op('pe', lambda e: e.matmul(out, lhsT=lhsT, rhs=rhs, start=start, stop=stop), reads=r, writes=w)

    def cp(eng, out, in_, r, w):
        if eng == 'act':
            kb.op('act', lambda e: e.copy(out=out, in_=in_), reads=r, writes=w)
        else:
            kb.op(eng, lambda e: e.tensor_copy(out=out, in_=in_), reads=r, writes=w)

    ident = cm[:, C_IDENT, :]
    ones = cm[:, C_ONES, :]

    kb.dma('sp', pp[:], pp_d, writes=['pp'])
    kb.dma('sp', modm[:].rearrange("p a b c -> p (a b c)"), modm_d, writes=['modm'])
    kb.dma('sp', lruw[:], lruw_d, writes=['lruw'])
    kb.dma('sp', cm[:], cm_d, writes=['cm'])
    kb.op('dve', lambda e: e.memset(cst[:, 0:1], 1.0), writes=['cst'])
    kb.op('dve', lambda e: e.memset(cst[:, 1:2], 0.0), writes=['cst'])
    kb.op('dve', lambda e: e.tensor_scalar_add(out=modm[:, :, 0, :], in0=modm[:, :, 0, :], scalar1=1.0),
          reads=['modm'], writes=['modm'])
    one_c = cst[:, 0:1]

    def stage_inproj():
        Wres = R[:, 0:30816].bitcast(BF16).rearrange("p (k c) -> p k c", k=16)
        x32 = R[:, 30816:34912].rearrange("p (k t) -> p k t", k=8)
        xm = R[:, 34912:39008].bitcast(BF16).rearrange("p (k t) -> p k t", k=16)
        wv = w_in.rearrange("(k p) c -> p k c", p=128)
        xv = xT.rearrange("(k p) t -> p k t", p=128)
        for k in range(16):
            kb.dma('pool', Wres[:, k, :], wv[:, k, :], writes=['Wres'])
        ei = 0
        for (t0, n, s) in M_BLOCKS:
            for half in range(2):
                kb.dma('sp', x32[:, :, :n], xv[:, half * 8:(half + 1) * 8, t0:t0 + n], writes=['x32'])
                for k8 in range(8):
                    k = half * 8 + k8
                    act(xm[:, k, :n], x32[:, k8, :n], AF.Identity, ['x32', 'modm'], ['xm%d' % k],
                        scale=modm[:, s, 0, k:k + 1], bias=modm[:, s, 1, k:k + 1])
            xmk = ['xm%d' % k for k in range(16)]
            for fc in range(NFM):
                pb = fc % 4
                for k in range(16):
                    mm(PS[pb][:, :n], Wres[:, k, fc * 128:(fc + 1) * 128], xm[:, k, :n], ['Wres', 'xm%d' % k], ['ps%d' % pb],
                       start=(k == 0), stop=(k == 15))
                e_ = ev[ei % 3]
                ek = 'ev%d' % (ei % 3)
                cp('act' if ei % 2 == 0 else 'dve', e_[:, :n], PS[pb][:, :n], ['ps%d' % pb], [ek])
                kb.dma('sp', Pfm[fc * 128:(fc + 1) * 128, t0:t0 + n], e_[:, :n], reads=[ek], writes=['Pfm_%d' % ei])
                ei += 1
            for tt_ in range(n // 128):
                a = tt_ * 128
                pb = 4 + (tt_ % 2)
                for k in range(16):
                    mm(PS[pb][:, :NTM], xm[:, k, a:a + 128], Wres[:, k, NFM * 128:NCOLS], ['Wres', 'xm%d' % k], ['ps%d' % pb],
                       start=(k == 0), stop=(k == 15))
                e_ = ev[ei % 3]
                ek = 'ev%d' % (ei % 3)
                cp('act' if ei % 2 == 0 else 'dve', e_[:, :NTM], PS[pb][:, :NTM], ['ps%d' % pb], [ek])
                kb.dma('sp', Ptm[t0 + a:t0 + a + 128, :], e_[:, :NTM], reads=[ek], writes=['Ptm_%d' % ei])
                ei += 1

    def dwconv(dst, src, wcol0, bcol, rk, wk):
        for (lo, hi) in SEGS:
            if bcol is None:
                ts('dve', dst[:, lo:hi], src[:, lo:hi], pp[:, wcol0 + 2:wcol0 + 3], None, ALU.mult, None, [rk, 'pp'], [wk])
            else:
                ts('dve', dst[:, lo:hi], src[:, lo:hi], pp[:, wcol0 + 2:wcol0 + 3], pp[:, bcol:bcol + 1], ALU.mult, ALU.add,
                   [rk, 'pp'], [wk])
            for k, o in ((0, -2), (1, -1), (3, 1)):
                a, b = max(lo, lo - o), min(hi, hi - o)
                stt('dve', dst[:, a:b], src[:, a + o:b + o], pp[:, wcol0 + k:wcol0 + k + 1], dst[:, a:b], ALU.mult, ALU.add,
                    [rk, 'pp', wk], [wk])

    BLK = [(i * 512, 512) for i in range(8)] + [(4096, 256)]

    def load_tm(dst3, c0, c1, wk):
        src = Ptm[:, c0:c1].rearrange("(c p) v -> p c v", p=128)
        for a in range(0, NCH, 8):
            b = min(NCH, a + 8)
            kb.dma('sp', dst3[:, a:b, :], src[:, a:b, :], reads=['Ptm'], writes=[wk])

    def stage_lru():
        bufs = [R[:, i * L:(i + 1) * L] for i in range(8)]
        xb, xc, gt, ra, ib, hf, hb, tp = bufs
        for cc in range(2):
            p0 = PP_LRU + cc * 11
            kb.dma('sp', xb, Pfm[cc * 128:(cc + 1) * 128, :], reads=['Pfm'], writes=['xb'])
            kb.dma('sp', gt, Pfm[(2 + cc) * 128:(3 + cc) * 128, :], reads=['Pfm'], writes=['gt'])
            dwconv(xc, xb, p0, p0 + 4, 'xb', 'xc')
            for d in range(2):
                act(tmpc[:, 0:1], pp[:, p0 + 9 + d:p0 + 10 + d], AF.Exp, ['pp'], ['tmpc'], scale=-1.0)
                act(tmpc[:, 0:1], tmpc[:, 0:1], AF.Ln, ['tmpc', 'cst'], ['tmpc'], bias=one_c)
                ts('dve', tmpc[:, 1:2], tmpc[:, 0:1], -8.0, None, ALU.mult, None, ['tmpc'], ['tmpc'])
                for (t0, n) in BLK:
                    mm(PS[0][:, :n], lruw[:, d * 4 + 0 + cc, :], xc[:, t0:t0 + n], ['lruw', 'xc'], ['ps0'])
                    mm(PS[1][:, :n], lruw[:, d * 4 + 2 + cc, :], xc[:, t0:t0 + n], ['lruw', 'xc'], ['ps1'])
                    act(ra[:, t0:t0 + n], PS[0][:, :n], AF.Sigmoid, ['ps0', 'pp'], ['ra'], bias=pp[:, p0 + 5 + d:p0 + 6 + d])
                    act(ib[:, t0:t0 + n], PS[1][:, :n], AF.Sigmoid, ['ps1', 'pp'], ['ib'], bias=pp[:, p0 + 7 + d:p0 + 8 + d])
                act(ra, ra, AF.Exp, ['ra', 'tmpc'], ['ra'], scale=tmpc[:, 1:2])
                tt('dve', tp, ra, ra, ALU.mult, ['ra'], ['tp'])
                ts('dve', tp, tp, -1.0, 1.0, ALU.mult, ALU.add, ['tp'], ['tp'])
                ts('dve', tp, tp, 0.0, None, ALU.max, None, ['tp'], ['tp'])
                act(tp, tp, AF.Sqrt, ['tp'], ['tp'])
                tt('pool', ib, ib, xc, ALU.mult, ['ib', 'xc'], ['ib'])
                tt('dve', ib, ib, tp, ALU.mult, ['ib', 'tp'], ['ib'])
                if d == 0:
                    kb.op('dve', lambda e: e.tensor_tensor_scan(out=hf, data0=ra, data1=ib, initial=0.0, op0=ALU.mult, op1=ALU.add),
                          reads=['ra', 'ib'], writes=['hf'])
                else:
                    kb.op('dve', lambda e: e.tensor_tensor_scan(out=hb[:, 0:CTX][:, ::-1], data0=ra[:, 0:CTX][:, ::-1],
                                                                data1=ib[:, 0:CTX][:, ::-1], initial=0.0, op0=ALU.mult, op1=ALU.add),
                          reads=['ra', 'ib'], writes=['hb'])
                    kb.op('dve', lambda e: e.tensor_tensor_scan(out=hb[:, CTX:NT][:, ::-1], data0=ra[:, CTX:NT][:, ::-1],
                                                                data1=ib[:, CTX:NT][:, ::-1], initial=hb[:, 0:1], op0=ALU.mult, op1=ALU.add),
                          reads=['ra', 'ib', 'hb'], writes=['hb'])
            tt('dve', hf, hf, hb, ALU.add, ['hf', 'hb'], ['hf'])
            tt('pool', tp, gt, gt, ALU.mult, ['gt'], ['tp'])
            ts('dve', tp, tp, 0.044715, 1.0, ALU.mult, ALU.add, ['tp'], ['tp'])
            tt('dve', tp, tp, gt, ALU.mult, ['tp', 'gt'], ['tp'])
            act(tp, tp, AF.Tanh, ['tp'], ['tp'], scale=0.7978845608028654)
            ts('dve', tp, tp, 1.0, 0.5, ALU.add, ALU.mult, ['tp'], ['tp'])
            tt('pool', tp, tp, gt, ALU.mult, ['tp', 'gt'], ['tp'])
            tt('dve', hf, hf, tp, ALU.mult, ['hf', 'tp'], ['hf'])
            kb.dma('sp', yo[cc * 128:(cc + 1) * 128, :], hf, reads=['hf'], writes=['yo'], is_out=True)

    sc_s = {n_: kb.sb("sc_" + n_, [128, 128]) for n_ in
            ['exprow', 'arg', 'decT', 'PT', 'qd', 'kw', 'vp', 'N', 'NT_', 'N2', 'N2T', 'X', 'kd', 'vm', 'vn']}
    sc_all = kb.sb("sc_all", [128, 5, NCH])
    Sst = kb.sb("Sst", [128, 128])

    def decay_scan(d, qT, kT, ktok, vfn, G, Yacc, dk, dvp, ysl, yfirst, keys, beta=None, vdyn=None, vdyn_key=None):
        tri = cm[:, C_TRIF + d, :]
        nm = cm[:, C_NMF + d, :]
        nst = cm[:, C_NSF + d, :]
        rq, rk_, rkt, rv, rG, wY = keys
        order = list(range(NCH)) if d == 0 else [1, 0] + list(range(NCH - 1, 1, -1))
        kb.op('dve', lambda e: e.memset(Sst[:], 0.0), writes=['Sst'])
        S = Sst[0:dk, 0:dvp]
        ex, arg, decT, PT_, qd, kw, vp, N_, NT_, N2, N2T, X, kd, vm, vn = [sc_s[n_] for n_ in
            ['exprow', 'arg', 'decT', 'PT', 'qd', 'kw', 'vp', 'N', 'NT_', 'N2', 'N2T', 'X', 'kd', 'vm', 'vn']]
        mm(PS[0][:, 0:NCH], tri, G[:, 0:NCH], [rG, 'cm'], ['ps0'])
        mm(PS[0][:, 64:64 + NCH], ones, G[:, 0:NCH], [rG, 'cm'], ['ps0'])
        cp('dve', sc_all[:, 0, :], PS[0][:, 0:NCH], ['ps0'], ['scall'])
        cp('dve', sc_all[:, 1, :], PS[0][:, 64:64 + NCH], ['ps0'], ['scall'])
        ts('dve', sc_all[:, 2, :], sc_all[:, 0, :], -1.0, None, ALU.mult, None, ['scall'], ['scall'])
        tt('dve', sc_all[:, 3, :], sc_all[:, 1, :], sc_all[:, 0, :], ALU.subtract, ['scall'], ['scall'])
        act(sc_all[:, 3, :], sc_all[:, 3, :], AF.Exp, ['scall'], ['scall'])
        act(sc_all[:, 4, :], sc_all[:, 1, :], AF.Exp, ['scall'], ['scall'])
        STEP = int(os.environ.get('M_STEP', '99'))
        if STEP <= 1:
            return
        for ci, c in enumerate(order[:int(os.environ.get('M_NCH', '99'))]):
            tok = slice(c * 128, (c + 1) * 128)
            gcol = G[:, c:c + 1]
            negcum = sc_all[:, 2, c:c + 1]
            wcol = sc_all[:, 3, c:c + 1]
            etot = sc_all[0:dk, 4, c:c + 1]
            mm(PS[0][:, 128:256], gcol.to_broadcast([128, 128]), tri, [rG, 'cm'], ['ps0'])
            act(ex[:], PS[0][:, 128:256], AF.Exp, ['ps0'], ['ex'])
            stt('dve', arg[:], PS[0][:, 128:256], negcum, nm, ALU.add, ALU.add, ['ps0', 'scall', 'cm'], ['arg'])
            act(decT[:], arg[:], AF.Exp, ['arg'], ['decT'])
            if STEP <= 2:
                continue
            if beta is not None:
                bcol = beta[:, c:c + 1]
                mm(PS[1][:, 0:128], kT[:, tok], kT[:, tok], [rk_], ['ps1'])
                stt('dve', N_[:], PS[1][:, 0:128], bcol, decT[:], ALU.mult, ALU.mult, ['ps1', rG, 'decT'], ['N'])
                tt('dve', N_[:], N_[:], nst, ALU.mult, ['N', 'cm'], ['N'])
                kb.op('pe', lambda e: e.transpose(out=PS[2][:, 0:128], in_=N_[:], identity=ident), reads=['N', 'cm'], writes=['ps2'])
                cp('act', NT_[:], PS[2][:, 0:128], ['ps2'], ['NT'])
                tt('dve', X[:], N_[:], ident, ALU.add, ['N', 'cm'], ['X'])
                curN, curNT, nxtN, nxtNT = N_, NT_, N2, N2T
                cN, cNT, nN, nNT = 'N', 'NT', 'N2', 'N2T'
                for it in range(1, 7):
                    mm(PS[1][:, 0:128], curNT[:], curN[:], [cN, cNT], ['ps1'])
                    mm(PS[2][:, 0:128], curN[:], curNT[:], [cN, cNT], ['ps2'])
                    cp('act', nxtN[:], PS[1][:, 0:128], ['ps1'], [nN])
                    cp('dve', nxtNT[:], PS[2][:, 0:128], ['ps2'], [nNT])
                    mm(PS[3][:, 0:128], nxtNT[:], X[:], [nNT, 'X'], ['ps3'])
                    tt('dve', X[:], X[:], PS[3][:, 0:128], ALU.add, ['X', 'ps3'], ['X'])
                    curN, curNT, nxtN, nxtNT = nxtN, nxtNT, curN, curNT
                    cN, cNT, nN, nNT = nN, nNT, cN, cNT
                tt('dve', kd[0:dk, :], kT[:, tok], ex[0:dk, :], ALU.mult, [rk_, 'ex'], ['kd'])
                mm(PS[4][:, 0:dvp], kd[0:dk, :], S, ['kd', 'Sst'], ['ps4'])
                tt('dve', vm[:, 0:dvp], vfn(c), PS[4][:, 0:dvp], ALU.subtract, [rv, 'ps4'], ['vm'])
                mm(PS[5][:, 0:dvp], X[:], vm[:, 0:dvp], ['X', 'vm'], ['ps5'])
                ts('dve', vn[:, 0:dvp], PS[5][:, 0:dvp], bcol, None, ALU.mult, None, ['ps5', rG], ['vn'])
                vc = vn[:, 0:dvp]
                vkey = 'vn'
            elif vdyn is not None:
                ts('dve', vp[:, 0:dvp], vfn(c), vdyn[:, c:c + 1], None, ALU.mult, None, [rv, vdyn_key], ['vp'])
                vc = vp[:, 0:dvp]
                vkey = 'vp'
            else:
                vc = vfn(c)
                vkey = rv
            SUB = os.environ.get('M_SUB', 'z')
            mm(PS[6][:, 0:128], kT[:, tok], qT[:, tok], [rk_, rq], ['ps6'])
            if SUB == 'a':
                continue
            tt('dve', PT_[:], PS[6][:, 0:128], decT[:], ALU.mult, ['ps6', 'decT'], ['PT'])
            if SUB == 'b':
                continue
            tt(os.environ.get('M_QDENG', 'pool'), qd[0:dk, :], qT[:, tok], ex[0:dk, :], ALU.mult, [rq, 'ex'] if os.environ.get('M_EXP1') is None else [rq], ['qd'])
            if STEP <= 3:
                continue
            mm(PS[7][0:dvp, 0:128], vc, PT_[:], [vkey, 'PT'], ['ps7'], start=True, stop=False)
            mm(PS[7][0:dvp, 0:128], S, qd[0:dk, :], ['Sst', 'qd'], ['ps7'], start=False, stop=True)
            if yfirst:
                cp('act', Yacc[ysl, tok], PS[7][ysl, 0:128], ['ps7'], [wY])
            else:
                tt('dve', Yacc[ysl, tok], Yacc[ysl, tok], PS[7][ysl, 0:128], ALU.add, [wY, 'ps7'], [wY])
            if STEP <= 4:
                continue
            ts('dve', kw[:, 0:dk], ktok[:, c, :], wcol, None, ALU.mult, None, [rkt, 'scall'], ['kw'])
            mm(PS[4][0:dk, 0:dvp], kw[:, 0:dk], vc, ['kw', vkey], ['ps4'])
            stt('dve', S, S, etot, PS[4][0:dk, 0:dvp], ALU.mult, ALU.add, ['Sst', 'scall', 'ps4'], ['Sst'])

    def to_tm(dst, src, rk, wk):
        for c in range(NCH):
            pb = 2 + (c % 2)
            kb.op('pe', lambda e: e.transpose(out=PS[pb][:, 0:128], in_=src[:, c * 128:(c + 1) * 128], identity=ident),
                  reads=[rk, 'cm'], writes=['ps%d' % pb])
            cp('act' if c % 2 == 0 else 'dve', dst[:, c, :], PS[pb][:, 0:128], ['ps%d' % pb], [wk])

    def fm_norm_gate(Y, gate, wcol, eps, rkY, rkg, normalize=True):
        for (t0, n) in BLK:
            e0, e1 = ev[0], ev[1]
            if normalize:
                act(e0[:, :n], Y[:, t0:t0 + n], AF.Square, [rkY], ['ev0'])
                mm(PS[0][:, :n], ones, e0[:, :n], ['cm', 'ev0'], ['ps0'])
                ts('dve', e0[:, :n], PS[0][:, :n], 1.0 / 128, eps, ALU.mult, ALU.add, ['ps0'], ['ev0'])
                act(e0[:, :n], e0[:, :n], AF.Sqrt, ['ev0'], ['ev0'])
                kb.op('dve', lambda e: e.reciprocal(out=e0[:, :n], in_=e0[:, :n]), reads=['ev0'], writes=['ev0'])
                stt('dve', Y[:, t0:t0 + n], Y[:, t0:t0 + n], wcol, e0[:, :n], ALU.mult, ALU.mult, [rkY, 'pp', 'ev0'], [rkY])
            act(e1[:, :n], gate[:, t0:t0 + n], AF.Silu, [rkg], ['ev1'])
            tt('dve', Y[:, t0:t0 + n], Y[:, t0:t0 + n], e1[:, :n], ALU.mult, [rkY, 'ev1'], [rkY])

    CUT = int(os.environ.get('M_CUT', '99'))

    def stage_ret():
        q, qs, k, ks, ktm, vtm, gT, Y = [R[:, i * L:(i + 1) * L] for i in range(8)]
        ktm3 = ktm.rearrange("p (c d) -> p c d", c=NCH)
        vtm3 = vtm.rearrange("p (c d) -> p c d", c=NCH)
        Gt = kb.sb("ret_G", [128, NCH])
        for hl in range(2):
            f0 = 4 + hl * 5
            for i, (buf, key) in enumerate([(q, 'q'), (qs, 'qs'), (k, 'k'), (ks, 'ks'), (gT, 'gT')]):
                kb.dma('sp', buf, Pfm[(f0 + i) * 128:(f0 + i + 1) * 128, :], reads=['Pfm'], writes=[key])
            load_tm(vtm3, hl * 128, (hl + 1) * 128, 'vtm')
            for (t0, n) in BLK:
                kb.dma('sp', ev[0][:, :n], ropec_d[:, t0:t0 + n], writes=['ev0'])
                kb.dma('sp', ev[1][:, :n], ropes_d[:, t0:t0 + n], writes=['ev1'])
                for a_, b_, ka, kb_ in ((q, qs, 'q', 'qs'), (k, ks, 'k', 'ks')):
                    tt('dve', a_[:, t0:t0 + n], a_[:, t0:t0 + n], ev[0][:, :n], ALU.mult, [ka, 'ev0'], [ka])
                    tt('pool', b_[:, t0:t0 + n], b_[:, t0:t0 + n], ev[1][:, :n], ALU.mult, [kb_, 'ev1'], [kb_])
                    tt('dve', a_[:, t0:t0 + n], a_[:, t0:t0 + n], b_[:, t0:t0 + n], ALU.add, [ka, kb_], [ka])
            kb.op('act', lambda e: e.mul(out=q, in_=q, mul=128.0 ** -0.5), reads=['q'], writes=['q'])
            if CUT <= 1:
                return
            to_tm(ktm3, k, 'k', 'ktm')
            if CUT <= 2:
                return
            for d in range(2):
                act(tmpc[:, 0:1], pp[:, PP_RETLD + d * 2 + hl:PP_RETLD + d * 2 + hl + 1], AF.Exp, ['pp'], ['tmpc'])
                ts('dve', Gt[:], tmpc[:, 0:1].to_broadcast([128, NCH]), -1.0, None, ALU.mult, None, ['tmpc'], ['retG'])
                decay_scan(d, q, k, ktm3, lambda c: vtm3[:, c, :], Gt, Y, 128, 128, slice(0, 128), d == 0,
                           ('q', 'k', 'ktm', 'vtm', 'retG', 'Y'))
                if CUT <= 3:
                    return
            if CUT <= 4:
                return
            fm_norm_gate(Y, gT, pp[:, PP_RETN + hl:PP_RETN + hl + 1], 1e-6, 'Y', 'gT')
            kb.dma('sp', yo[256 + hl * 128:256 + (hl + 1) * 128, :], Y, reads=['Y'], writes=['yo'], is_out=True)

    def softplus_cols(out, x, bcol, rk, wk):
        act(out, x, AF.Exp, [rk, 'pp'], [wk], bias=bcol)
        act(out, out, AF.Ln, [wk, 'cst'], [wk], bias=one_c)

    def stage_ssd():
        xc0, xc1, BBc, CCc = [R[:, i * L:(i + 1) * L] for i in range(4)]
        src = R[:, 4 * L:5 * L]
        xtm = R[:, 5 * L:7 * L].rearrange("p (c d) -> p c d", c=NCH)
        btm = R[:, 7 * L:8 * L].rearrange("p (c d) -> p c d", c=NCH)
        Y0 = R[:, 8 * L:9 * L]
        Y1 = R[:, 4 * L:5 * L]
        dtr = kb.sb("ssd_dtr", [128, NCH, 4])
        dtv = kb.sb("ssd_dt", [128, NCH])
        Gt = kb.sb("ssd_G", [128, NCH])
        load_tm(dtr[:], 256, 260, 'dtr')
        for i, dst in enumerate([xc0, xc1, BBc, CCc]):
            kb.dma('sp', src, Pfm[(16 + i) * 128:(17 + i) * 128, :], reads=['Pfm'], writes=['src'])
            dwconv(dst, src, PP_SSDCW + i * 4, PP_SSDCB + i, 'src', 'c%d' % i)
            act(dst, dst, AF.Silu, ['c%d' % i], ['c%d' % i])
        kb.barrier()
        to_tm(xtm[:, :, 0:128], xc0, 'c0', 'xtm')
        to_tm(xtm[:, :, 128:256], xc1, 'c1', 'xtm')
        to_tm(btm, BBc, 'c2', 'btm')
        for h in range(4):
            Y = Y0 if h < 2 else Y1
            ysl = slice((h % 2) * 64, (h % 2) * 64 + 64)
            pair = slice((h // 2) * 128, (h // 2) * 128 + 128)
            for d in range(2):
                ci = d * 4 + h
                softplus_cols(dtv[:], dtr[:, :, h], pp[:, PP_SSDDTB + ci:PP_SSDDTB + ci + 1], 'dtr', 'dtv')
                act(tmpc[:, 0:1], pp[:, PP_SSDAL + ci:PP_SSDAL + ci + 1], AF.Exp, ['pp'], ['tmpc'])
                ts('dve', tmpc[:, 1:2], tmpc[:, 0:1], -1.0, None, ALU.mult, None, ['tmpc'], ['tmpc'])
                ts('dve', Gt[:], dtv[:], tmpc[:, 1:2], None, ALU.mult, None, ['dtv', 'tmpc'], ['ssdG'])
                decay_scan(d, CCc[0:64, :], BBc[0:64, :], btm[:, :, 0:64], lambda c: xtm[:, c, pair], Gt,
                           Y, 64, 128, ysl, d == 0, ('c3', 'c2', 'btm', 'xtm', 'ssdG', 'Y%d' % (h // 2)), vdyn=dtv, vdyn_key='dtv')
        for cc, (Y, xcc) in enumerate([(Y0, xc0), (Y1, xc1)]):
            stt('dve', Y, xcc, pp[:, PP_SSDD + cc:PP_SSDD + cc + 1], Y, ALU.mult, ALU.add, ['c%d' % cc, 'pp', 'Y%d' % cc], ['Y%d' % cc])
            kb.dma('sp', BBc, Pfm[(14 + cc) * 128:(15 + cc) * 128, :], reads=['Pfm'], writes=['c2'])
            fm_norm_gate(Y, BBc, None, 0.0, 'Y%d' % cc, 'c2', normalize=False)
            kb.dma('sp', yo[512 + cc * 128:512 + (cc + 1) * 128, :], Y, reads=['Y%d' % cc], writes=['yo'], is_out=True)

    def stage_gdn():
        q, k, v, src, ktm, vtm, Y = [R[:, i * L:(i + 1) * L] for i in range(7)]
        ktm3 = ktm.rearrange("p (c d) -> p c d", c=NCH)
        vtm3 = vtm.rearrange("p (c d) -> p c d", c=NCH)
        abr = kb.sb("gdn_ab", [128, NCH, 8])
        Gt = kb.sb("gdn_G", [128, NCH])
        Bt = kb.sb("gdn_B", [128, NCH])
        load_tm(abr[:], 260, 268, 'abr')
        for hl in range(2):
            for i, (dst, key) in enumerate([(q, 'q'), (k, 'k'), (v, 'v')]):
                fc = 20 + i * 2 + hl
                kb.dma('sp', src, Pfm[fc * 128:(fc + 1) * 128, :], reads=['Pfm'], writes=['src'])
                dwconv(dst, src, PP_GDNCW + (i * 2 + hl) * 4, None, 'src', key)
                act(dst, dst, AF.Silu, [key], [key])
            for buf, key, post in ((q, 'q', 128.0 ** -0.5), (k, 'k', 1.0)):
                for (t0, n) in BLK:
                    act(ev[0][:, :n], buf[:, t0:t0 + n], AF.Square, [key], ['ev0'])
                    mm(PS[0][:, :n], ones, ev[0][:, :n], ['cm', 'ev0'], ['ps0'])
                    ts('dve', ev[0][:, :n], PS[0][:, :n], 1e-6, None, ALU.add, None, ['ps0'], ['ev0'])
                    act(ev[0][:, :n], ev[0][:, :n], AF.Sqrt, ['ev0'], ['ev0'])
                    kb.op('dve', lambda e: e.reciprocal(out=ev[0][:, :n], in_=ev[0][:, :n]), reads=['ev0'], writes=['ev0'])
                    stt('dve', buf[:, t0:t0 + n], buf[:, t0:t0 + n], post, ev[0][:, :n], ALU.mult, ALU.mult, [key, 'ev0'], [key])
            to_tm(ktm3, k, 'k', 'ktm')
            to_tm(vtm3, v, 'v', 'vtm')
            for d in range(2):
                ci = d * 2 + hl
                softplus_cols(Gt[:], abr[:, :, ci], pp[:, PP_GDNDTB + ci:PP_GDNDTB + ci + 1], 'abr', 'gdnG')
                act(tmpc[:, 0:1], pp[:, PP_GDNAL + ci:PP_GDNAL + ci + 1], AF.Exp, ['pp'], ['tmpc'])
                ts('dve', tmpc[:, 1:2], tmpc[:, 0:1], -1.0, None, ALU.mult, None, ['tmpc'], ['tmpc'])
                ts('dve', Gt[:], Gt[:], tmpc[:, 1:2], None, ALU.mult, None, ['gdnG', 'tmpc'], ['gdnG'])
                act(Bt[:], abr[:, :, 4 + ci], AF.Sigmoid, ['abr'], ['gdnG'])
                decay_scan(d, q, k, ktm3, lambda c: vtm3[:, c, :], Gt, Y, 128, 128, slice(0, 128), d == 0,
                           ('q', 'k', 'ktm', 'vtm', 'gdnG', 'Y'), beta=Bt)
            kb.dma('sp', src, Pfm[(26 + hl) * 128:(27 + hl) * 128, :], reads=['Pfm'], writes=['src'])
            fm_norm_gate(Y, src, pp[:, PP_GDNN:PP_GDNN + 1], 1e-6, 'Y', 'src')
            kb.dma('sp', yo[768 + hl * 128:768 + (hl + 1) * 128, :], Y, reads=['Y'], writes=['yo'], is_out=True)

    for st, fn in (('in', stage_inproj), ('lru', stage_lru), ('ret', stage_ret), ('ssd', stage_ssd), ('gdn', stage_gdn)):
        if st in stages:
            fn()
            kb.barrier()
    kb.finish()
    return nc


def _pp(v):
    return np.ascontiguousarray(np.asarray(v, dtype=np.float32).reshape(-1, 128).T)


def _bc(x):
    return np.full((128, 1), x, dtype=np.float32)


def _m_cols(hh):
    ar = np.arange
    cols = []
    cols += list(0 + hh * 256 + ar(256))
    cols += list(512 + hh * 256 + ar(256))
    perm = np.concatenate([ar(32) + 32, ar(32), ar(32) + 96, ar(32) + 64])
    for hl in range(2):
        h = hh * 2 + hl
        qb, kb_, gb = 1024 + h * 128, 1536 + h * 128, 2560 + h * 128
        cols += list(qb + ar(128)) + list(qb + perm) + list(kb_ + ar(128)) + list(kb_ + perm) + list(gb + ar(128))
    cols += list(3072 + hh * 256 + ar(256))
    cols += list(3584 + hh * 256 + ar(256))
    cols += list(4096 + hh * 64 + ar(64)) * 2
    cols += list(4224 + hh * 64 + ar(64)) * 2
    for base in (4360, 4872, 5384):
        for hl in range(2):
            cols += list(base + (hh * 2 + hl) * 128 + ar(128))
    for hl in range(2):
        cols += list(5896 + (hh * 2 + hl) * 128 + ar(128))
    for hl in range(2):
        cols += list(2048 + (hh * 2 + hl) * 128 + ar(128))
    cols += list(4352 + hh * 4 + ar(4))
    for base in (6408, 6416):
        for d in range(2):
            for hl in range(2):
                cols.append(base + d * 4 + hh * 2 + hl)
    cols = np.array(cols)
    assert cols.shape[0] == NCOLS
    return cols


def _m_consts():
    i = np.arange(128)
    J, I = np.meshgrid(i, i, indexing='ij')
    cm = np.zeros((128, 8, 128), np.float32)
    cm[:, C_IDENT] = (J == I)
    cm[:, C_ONES] = 1.0
    cm[:, C_TRIF] = (J <= I)
    cm[:, C_TRIR] = (J >= I)
    cm[:, C_NMF] = np.where(I >= J, 0.0, -30000.0)
    cm[:, C_NMR] = np.where(I <= J, 0.0, -30000.0)
    cm[:, C_NSF] = np.where(I > J, -1.0, 0.0)
    cm[:, C_NSR] = np.where(I < J, -1.0, 0.0)
    t = np.arange(SEQ)
    rows = (t // 64).astype(np.float64)
    colsp = (t % 64).astype(np.float64)
    inv = 10000.0 ** (-np.arange(0, 64, 2, dtype=np.float64) / 64)
    rc = np.ones((128, NT), np.float64)
    rsn = np.zeros((128, NT), np.float64)
    for half, pos in ((0, rows), (1, colsp)):
        ang = pos[None, :] * inv[:, None]
        o = half * 64
        rc[o:o + 32, CTX:] = np.cos(ang)
        rc[o + 32:o + 64, CTX:] = np.cos(ang)
        rsn[o:o + 32, CTX:] = -np.sin(ang)
        rsn[o + 32:o + 64, CTX:] = np.sin(ang)
    return cm, rc.astype(np.float32), rsn.astype(np.float32)


def _m_params(inp, l, hh):
    pp = np.zeros((128, NPP), np.float32)
    for cc in range(2):
        ch = hh * 256 + cc * 128 + np.arange(128)
        p0 = PP_LRU + cc * 11
        for k in range(4):
            pp[:, p0 + k] = inp['lru_conv_w'][l][k, ch]
        pp[:, p0 + 4] = inp['lru_conv_b'][l][ch]
        for d in range(2):
            pp[:, p0 + 5 + d] = inp['lru_gate_a_b'][l][d, ch]
            pp[:, p0 + 7 + d] = inp['lru_gate_x_b'][l][d, ch]
            pp[:, p0 + 9 + d] = inp['lru_lambda'][l][d, ch]
    for hl in range(2):
        h = hh * 2 + hl
        pp[:, PP_RETN + hl] = inp['ret_norm_w'][l][h * 128:(h + 1) * 128]
        for d in range(2):
            pp[:, PP_RETLD + d * 2 + hl] = inp['ret_log_decay'][l][d, h]
            pp[:, PP_GDNDTB + d * 2 + hl] = inp['gdn_dt_bias'][l][d, h]
            pp[:, PP_GDNAL + d * 2 + hl] = inp['gdn_a_log'][l][d, h]
    ssd_ch = [hh * 256 + np.arange(128), hh * 256 + 128 + np.arange(128),
              512 + hh * 64 + np.tile(np.arange(64), 2), 640 + hh * 64 + np.tile(np.arange(64), 2)]
    for i, ch in enumerate(ssd_ch):
        for k in range(4):
            pp[:, PP_SSDCW + i * 4 + k] = inp['ssd_conv_w'][l][k, ch]
        pp[:, PP_SSDCB + i] = inp['ssd_conv_b'][l][ch]
    for cc in range(2):
        pp[:, PP_SSDD + cc] = inp['ssd_d'][l][hh * 4 + cc * 2 + np.arange(128) // 64]
    for d in range(2):
        for h in range(4):
            pp[:, PP_SSDDTB + d * 4 + h] = inp['ssd_dt_bias'][l][d, hh * 4 + h]
            pp[:, PP_SSDAL + d * 4 + h] = inp['ssd_a_log'][l][d, hh * 4 + h]
    for i in range(3):
        for hl in range(2):
            ch = i * 512 + (hh * 2 + hl) * 128 + np.arange(128)
            for k in range(4):
                pp[:, PP_GDNCW + (i * 2 + hl) * 4 + k] = inp['gdn_conv_w'][l][k, ch]
    pp[:, PP_GDNN] = inp['gdn_norm_w'][l]
    lruw = np.zeros((128, 8, 128), np.float32)
    for d in range(2):
        for ai, name in enumerate(('lru_gate_a_w', 'lru_gate_x_w')):
            for cc in range(2):
                for j in range(2):
                    g = hh * 4 + cc * 2 + j
                    lruw[j * 64:(j + 1) * 64, d * 4 + ai * 2 + cc, j * 64:(j + 1) * 64] = inp[name][l][d, g]
    return pp, lruw


_NC_CACHE = {}


def _get_nc(name):
    if name not in _NC_CACHE:
        _NC_CACHE[name] = {'A': build_A, 'M': build_M, 'T': build_T}[name]()
    return _NC_CACHE[name]


def _launch(name, in_maps):
    nc = {'A': build_A, 'M': build_M, 'T': build_T}[name]()
    res = run_bass_kernel_spmd(nc, in_maps, core_ids=list(range(NCORES)))
    return res.results


def run_A(inp):
    cv = np.concatenate([np.asarray(inp['c'], np.float32), np.asarray(inp['c_ctx'], np.float32)[None]], 0)
    cvT = np.ascontiguousarray(cv.reshape(5, 16, 128).transpose(2, 1, 0))
    maps = []
    for c in range(NCORES):
        l, half = c // 2, c % 2
        maps.append({"w": np.ascontiguousarray(inp['w_ada'][l][:, half * 6144:(half + 1) * 6144]),
                     "bias": _pp(inp['b_ada'][l][half * 6144:(half + 1) * 6144]), "cvT": cvT})
    res = _launch('A', maps)
    mod = np.zeros((DEPTH, 5, 6 * D), np.float32)
    for c in range(NCORES):
        l, half = c // 2, c % 2
        o = res[c]["out"]
        mod[l][:, half * 6144:(half + 1) * 6144] = o.transpose(2, 1, 0).reshape(5, 6144)
    return mod


def m_in_maps(inp, l, mod_l, xTs, consts):
    cm, rc, rsn = consts
    maps = []
    per_hh = []
    for hh in range(2):
        cols = _m_cols(hh)
        w = np.ascontiguousarray(inp['w_in'][l][:, cols])
        pp, lruw = _m_params(inp, l, hh)
        per_hh.append((w, pp, lruw))
    for c in range(NCORES):
        b, hh = c // 2, c % 2
        w, pp, lruw = per_hh[hh]
        modm = np.zeros((128, 2, 2, 16), np.float32)
        for si, s in enumerate((4, b)):
            modm[:, si, 0, :] = _pp(mod_l[s][D:2 * D])
            modm[:, si, 1, :] = _pp(mod_l[s][0:D])
        maps.append({"xT": xTs[b], "modm": modm.reshape(128, -1), "w_in": w, "pp": pp, "lruw": lruw,
                     "cm": cm, "ropec": rc, "ropes": rsn})
    return maps


def t_in_maps(inp, l, mod_l, xTs, ycTs, ident):
    ew1 = np.concatenate([inp['exp_w1'][l], inp['sh_w1'][l][None]], 0)
    ew3 = np.concatenate([inp['exp_w3'][l], inp['sh_w3'][l][None]], 0)
    ew2 = np.concatenate([inp['exp_w2'][l], inp['sh_w2'][l][None]], 0)
    lnp = np.concatenate([_pp(inp['ln1_w'][l]), _pp(inp['ln1_b'][l]), _pp(inp['ln2_w'][l]), _pp(inp['ln2_b'][l]),
                          _pp(inp['ssd_norm_w'][l])], axis=1)
    rb = np.ascontiguousarray(np.broadcast_to(np.asarray(inp['router_bias'][l], np.float32), (128, NE)))
    w_out = np.ascontiguousarray(inp['w_out'][l])
    rw = np.ascontiguousarray(inp['router_w'][l])
    maps = []
    for c in range(NCORES):
        b, h2 = c // 2, c % 2
        tok = np.concatenate([h2 * 128 + np.arange(128), CTX + h2 * 2048 + np.arange(2048)])
        modp = np.zeros((128, 2, 4, 16), np.float32)
        for si, s in enumerate((4, b)):
            modp[:, si, 0, :] = _pp(mod_l[s][2 * D:3 * D])
            modp[:, si, 1, :] = _pp(mod_l[s][4 * D:5 * D])
            modp[:, si, 2, :] = _pp(mod_l[s][3 * D:4 * D])
            modp[:, si, 3, :] = _pp(mod_l[s][5 * D:6 * D])
        maps.append({"ycT": np.ascontiguousarray(ycTs[b][:, tok]), "xT": np.ascontiguousarray(xTs[b][:, tok]),
                     "modp": modp.reshape(128, -1), "lnp": lnp, "w_out": w_out, "rw": rw, "rb": rb,
                     "ew1": ew1, "ew3": ew3, "ew2": ew2, "ident": ident})
    return maps


def assemble_yc(res):
    ycTs = []
    for b in range(NB):
        y0, y1 = res[2 * b]["yo"], res[2 * b + 1]["yo"]
        parts = []
        for m in range(4):
            parts += [y0[m * 256:(m + 1) * 256], y1[m * 256:(m + 1) * 256]]
        ycTs.append(np.ascontiguousarray(np.concatenate(parts, 0)))
    return ycTs


def assemble_x(res):
    xTs = []
    for b in range(NB):
        x = np.empty((D, NT), np.float32)
        for h2 in range(2):
            o = res[2 * b + h2]["x2T"]
            x[:, h2 * 128:(h2 + 1) * 128] = o[:, 0:128]
            x[:, CTX + h2 * 2048:CTX + (h2 + 1) * 2048] = o[:, 128:]
        xTs.append(x)
    return xTs


def kernel(**inp):
    inp = {k: np.asarray(v) for k, v in inp.items()}
    consts = _m_consts()
    ident = np.eye(128, dtype=np.float32)
    mod = run_A(inp)
    xTs = [np.ascontiguousarray(np.concatenate([inp['ctx'][b], inp['x'][b]], 0).T.astype(np.float32)) for b in range(NB)]
    for l in range(DEPTH):
        resM = _launch('M', m_in_maps(inp, l, mod[l], xTs, consts))
        ycTs = assemble_yc(resM)
        del resM
        resT = _launch('T', t_in_maps(inp, l, mod[l], xTs, ycTs, ident))
        xTs = assemble_x(resT)
        del resT
    out = np.stack([np.ascontiguousarray(xTs[b][:, CTX:].T) for b in range(NB)], 0)
    return out.astype(np.float32)
```
